# Optimizing a Trainium2 kernel written in Bass

```python
import math
import jax
import jax.numpy as jnp
from jax import lax
import numpy as np

D_MODEL = 1024
BATCH = 1
SEQ = 16384
DEPTH = 2

HEAD_DIM = 64
HEADS_PER_MIXER = 4
N_MIXERS = 4
N_HEADS = HEADS_PER_MIXER * N_MIXERS
GROUP_WIDTH = HEADS_PER_MIXER * HEAD_DIM
MIX_WIDTH = N_HEADS * HEAD_DIM
MLA_Q_RANK = 256
MLA_KV_RANK = 128
MLA_NOPE = 64
MLA_ROPE = 32
MLA_V = HEAD_DIM
ROPE_THETA = 10000.0
QBLOCK = 128
MOBA_BLOCK = 256
MOBA_TOPK = 3
DIL_PATTERNS = ((128, 1), (512, 4), (2048, 16))
N_EXPERTS = 32
TOP_K = 4
D_EXPERT = 1024
SWIGLU_LIMIT = 7.0
SWIGLU_ALPHA = 1.702
EXPERT_BLOCK = 128
NORM_EPS = 1e-6
NEG_INF = -1e30
IN_COLS = MLA_Q_RANK + MLA_KV_RANK + MLA_ROPE + 3 * GROUP_WIDTH + HEADS_PER_MIXER + 6 * GROUP_WIDTH

kernel_name = 'hybrid_parallel_heads_mla_fox_moba_dilated_moe'


def rms_norm(x, g):
    xf = x.astype(jnp.float32)
    y = xf * lax.rsqrt(jnp.mean(xf * xf, axis=-1, keepdims=True) + NORM_EPS)
    return (y * g.astype(jnp.float32)).astype(x.dtype)


def alibi_slopes(n):
    return jnp.asarray(2.0 ** (-8.0 * np.arange(1, n + 1) / n), dtype=jnp.float32)


def rope(x, pos):
    half = x.shape[-1] // 2
    inv = 1.0 / (ROPE_THETA ** (jnp.arange(half, dtype=jnp.float32) / half))
    ang = pos.astype(jnp.float32)[:, None] * inv[None, :]
    cos = jnp.cos(ang)[None, :, None, :]
    sin = jnp.sin(ang)[None, :, None, :]
    xf = x.astype(jnp.float32)
    x1, x2 = xf[..., :half], xf[..., half:]
    return jnp.concatenate([x1 * cos - x2 * sin, x2 * cos + x1 * sin], axis=-1).astype(x.dtype)


def pad_seq(a, mult, axis):
    n = a.shape[axis]
    target = -(-n // mult) * mult
    if target == n:
        return a
    widths = [(0, 0)] * a.ndim
    widths[axis] = (0, target - n)
    return jnp.pad(a, widths)


def causal_block_attention(q, k, v, scale, decay_cum=None):
    B, H, S, dq = q.shape
    nb = S // QBLOCK
    qb = q.reshape(B, H, nb, QBLOCK, dq).transpose(2, 0, 1, 3, 4)
    k_pos = jnp.arange(S)

    def one_block(args):
        qi, bi = args
        s = jnp.einsum('bhqd,bhkd->bhqk', qi, k).astype(jnp.float32) * scale
        q_pos = bi * QBLOCK + jnp.arange(QBLOCK)
        if decay_cum is not None:
            fq = lax.dynamic_slice_in_dim(decay_cum, bi * QBLOCK, QBLOCK, axis=2)
            s = s + fq[..., :, None] - decay_cum[..., None, :]
        s = jnp.where(k_pos[None, :] <= q_pos[:, None], s, NEG_INF)
        p = jax.nn.softmax(s, axis=-1)
        return jnp.einsum('bhqk,bhkd->bhqd', p.astype(v.dtype), v)

    out = lax.map(one_block, (qb, jnp.arange(nb)))
    return out.transpose(1, 2, 0, 3, 4).reshape(B, H, S, v.shape[-1])


def moba_attention(q, k, v, slopes):
    B, H, S, d = q.shape
    scale = d ** -0.5
    q, k, v = (pad_seq(a, MOBA_BLOCK, 2) for a in (q, k, v))
    Sp = q.shape[2]
    nblk = Sp // MOBA_BLOCK
    topk = min(MOBA_TOPK, nblk)
    nq = Sp // QBLOCK
    kb = k.reshape(B, H, nblk, MOBA_BLOCK, d)
    vb = v.reshape(B, H, nblk, MOBA_BLOCK, d)
    k_mean = jnp.mean(kb.astype(jnp.float32), axis=3)
    own = jnp.arange(Sp) // MOBA_BLOCK
    past = jnp.arange(nblk)[None, :] < own[:, None]
    gate = jnp.einsum('bhsd,bhnd->bhsn', q.astype(jnp.float32), k_mean)
    gate = jnp.where(past, gate, NEG_INF)
    _, idx = lax.top_k(gate, topk)
    valid = idx < own[:, None]
    chunk = lambda a: a.reshape(B, H, nq, QBLOCK, a.shape[-1]).transpose(2, 0, 1, 3, 4)
    b_ix = jnp.arange(B)[:, None, None, None]
    h_ix = jnp.arange(H)[None, :, None, None]
    rel = jnp.arange(MOBA_BLOCK)
    sl = slopes.astype(jnp.float32)
    n_sel = topk * MOBA_BLOCK

    def one_chunk(args):
        qc, idxc, validc, ci = args
        t = ci * QBLOCK + jnp.arange(QBLOCK)
        kg = kb[b_ix, h_ix, idxc]
        vg = vb[b_ix, h_ix, idxc]
        kpos = idxc[..., None] * MOBA_BLOCK + rel
        s_sel = jnp.einsum('bhqd,bhqnjd->bhqnj', qc, kg).astype(jnp.float32) * scale
        s_sel = s_sel - sl[None, :, None, None, None] * (t[:, None, None] - kpos).astype(jnp.float32)
        s_sel = jnp.where(validc[..., None], s_sel, NEG_INF)
        start = (ci * QBLOCK) // MOBA_BLOCK * MOBA_BLOCK
        ko = lax.dynamic_slice_in_dim(k, start, MOBA_BLOCK, axis=2)
        vo = lax.dynamic_slice_in_dim(v, start, MOBA_BLOCK, axis=2)
        kpos_o = start + rel
        s_own = jnp.einsum('bhqd,bhjd->bhqj', qc, ko).astype(jnp.float32) * scale
        s_own = s_own - sl[None, :, None, None] * (t[:, None] - kpos_o[None, :]).astype(jnp.float32)
        s_own = jnp.where(kpos_o[None, :] <= t[:, None], s_own, NEG_INF)
        s_all = jnp.concatenate([s_sel.reshape(B, H, QBLOCK, n_sel), s_own], axis=-1)
        p = jax.nn.softmax(s_all, axis=-1).astype(v.dtype)
        p_sel = p[..., :n_sel].reshape(B, H, QBLOCK, topk, MOBA_BLOCK)
        return (jnp.einsum('bhqnj,bhqnjd->bhqd', p_sel, vg)
                + jnp.einsum('bhqj,bhjd->bhqd', p[..., n_sel:], vo))

    out = lax.map(one_chunk, (chunk(q), chunk(idx), chunk(valid), jnp.arange(nq)))
    return out.transpose(1, 2, 0, 3, 4).reshape(B, H, Sp, d)[:, :, :S]


def dilated_attention(q, k, v, slopes):
    B, H, S, d = q.shape
    scale = d ** -0.5
    outs, lses = [], []
    for window, r in DIL_PATTERNS:
        span = window // r
        Sp = -(-S // (r * span)) * (r * span)
        L = Sp // r
        nb = L // span

        def to_sub(a):
            dd = a.shape[-1]
            a = pad_seq(a, r * span, 2)
            return a.reshape(B, H, L, r, dd).transpose(0, 1, 3, 2, 4).reshape(B, H, r, nb, span, dd)

        def with_prev(a):
            prev = jnp.concatenate([jnp.zeros_like(a[:, :, :, :1]), a[:, :, :, :-1]], axis=3)
            return jnp.concatenate([prev, a], axis=4)

        qs = to_sub(q)
        kc = with_prev(to_sub(k))
        vc = with_prev(to_sub(v))
        i = jnp.arange(span)
        j = jnp.arange(2 * span)
        dist = i[:, None] + span - j[None, :]
        valid = (dist >= 0) & (dist <= span)
        valid = valid[None] & ((jnp.arange(nb)[:, None, None] > 0) | (j[None, None, :] >= span))
        s = jnp.einsum('bhrnqd,bhrnjd->bhrnqj', qs, kc).astype(jnp.float32) * scale
        s = s - slopes.astype(jnp.float32)[None, :, None, None, None, None] * (dist * r).astype(jnp.float32)
        s = jnp.where(valid, s, NEG_INF)
        lse = jax.nn.logsumexp(s, axis=-1)
        p = jnp.exp(s - lse[..., None])
        o = jnp.einsum('bhrnqj,bhrnjd->bhrnqd', p.astype(v.dtype), vc)
        dv = v.shape[-1]
        outs.append(o.reshape(B, H, r, L, dv).transpose(0, 1, 3, 2, 4).reshape(B, H, Sp, dv)[:, :, :S])
        lses.append(lse.reshape(B, H, r, L).transpose(0, 1, 3, 2).reshape(B, H, Sp)[:, :, :S])
    w = jax.nn.softmax(jnp.stack(lses), axis=0)
    o = jnp.sum(w[..., None] * jnp.stack(outs).astype(jnp.float32), axis=0)
    return o.astype(q.dtype)


def token_mixers(h, w_in, mla_cq_g, mla_w_uq, mla_ckv_g, mla_w_ukv, mla_q_g, mla_k_g,
                 fox_q_g, fox_k_g, fox_b_f, moba_q_g, moba_k_g, dil_q_g, dil_k_g, w_out):
    B, S, _ = h.shape
    Hm = HEADS_PER_MIXER
    proj = h @ w_in
    sizes = [MLA_Q_RANK, MLA_KV_RANK, MLA_ROPE,
             GROUP_WIDTH, GROUP_WIDTH, GROUP_WIDTH, Hm,
             GROUP_WIDTH, GROUP_WIDTH, GROUP_WIDTH,
             GROUP_WIDTH, GROUP_WIDTH, GROUP_WIDTH]
    splits = [int(s) for s in np.cumsum(sizes)[:-1]]
    cq, ckv, kr, fq, fk, fv, flog, mq, mk, mv, dq, dk, dv = jnp.split(proj, splits, axis=-1)
    pos = jnp.arange(S)
    heads = lambda a: a.reshape(B, S, Hm, -1)
    bhsd = lambda a: a.transpose(0, 2, 1, 3)

    q_a = heads(rms_norm(cq, mla_cq_g) @ mla_w_uq)
    kv_a = heads(rms_norm(ckv, mla_ckv_g) @ mla_w_ukv)
    k_a = jnp.concatenate([kv_a[..., :MLA_NOPE],
                           jnp.broadcast_to(kr[:, :, None, :], (B, S, Hm, MLA_ROPE))], axis=-1)
    v_a = kv_a[..., MLA_NOPE:]
    q_a = rms_norm(q_a, mla_q_g)
    k_a = rms_norm(k_a, mla_k_g)
    q_a = jnp.concatenate([q_a[..., :MLA_NOPE], rope(q_a[..., MLA_NOPE:], pos)], axis=-1)
    k_a = jnp.concatenate([k_a[..., :MLA_NOPE], rope(k_a[..., MLA_NOPE:], pos)], axis=-1)
    o_a = causal_block_attention(bhsd(q_a), bhsd(k_a), bhsd(v_a), (MLA_NOPE + MLA_ROPE) ** -0.5)

    log_f = jax.nn.log_sigmoid((flog + fox_b_f).astype(jnp.float32))
    decay_cum = jnp.cumsum(log_f, axis=1).transpose(0, 2, 1)
    o_b = causal_block_attention(bhsd(rms_norm(heads(fq), fox_q_g)), bhsd(rms_norm(heads(fk), fox_k_g)),
                                 bhsd(heads(fv)), HEAD_DIM ** -0.5, decay_cum)

    slopes = alibi_slopes(2 * Hm)
    o_c = moba_attention(bhsd(rms_norm(heads(mq), moba_q_g)), bhsd(rms_norm(heads(mk), moba_k_g)),
                         bhsd(heads(mv)), slopes[1::2])
    o_d = dilated_attention(bhsd(rms_norm(heads(dq), dil_q_g)), bhsd(rms_norm(heads(dk), dil_k_g)),
                            bhsd(heads(dv)), slopes[0::2])

    o = jnp.concatenate([o_a, o_b.astype(o_a.dtype), o_c.astype(o_a.dtype), o_d.astype(o_a.dtype)], axis=1)
    o = o.transpose(0, 2, 1, 3).reshape(B, S, MIX_WIDTH)
    return o @ w_out


def moe_ffn(h, router_w, router_b, w1, b1, w2, b2):
    B, S, D = h.shape
    n_tok = B * S
    hf = h.reshape(n_tok, D)
    logits = (hf @ router_w + router_b).astype(jnp.float32)
    top_val, top_idx = lax.top_k(logits, TOP_K)
    gates = jax.nn.softmax(top_val, axis=-1)
    m = n_tok * TOP_K
    e_flat = top_idx.reshape(m)
    tok_flat = jnp.repeat(jnp.arange(n_tok, dtype=jnp.int32), TOP_K)
    g_flat = gates.reshape(m)
    order = jnp.argsort(e_flat)
    e_sorted = e_flat[order]
    counts = jnp.bincount(e_flat, length=N_EXPERTS)
    padded = (counts + EXPERT_BLOCK - 1) // EXPERT_BLOCK * EXPERT_BLOCK
    pad_end = jnp.cumsum(padded)
    pad_start = pad_end - padded
    start = jnp.cumsum(counts) - counts
    dest = pad_start[e_sorted] + jnp.arange(m) - start[e_sorted]
    m_pad = -(-m // EXPERT_BLOCK) * EXPERT_BLOCK + N_EXPERTS * EXPERT_BLOCK
    row_tok = jnp.zeros((m_pad,), jnp.int32).at[dest].set(tok_flat[order])
    row_gate = jnp.zeros((m_pad,), jnp.float32).at[dest].set(g_flat[order])
    n_blk = m_pad // EXPERT_BLOCK
    blk_expert = jnp.minimum(jnp.searchsorted(pad_end, jnp.arange(n_blk) * EXPERT_BLOCK, side='right'),
                             N_EXPERTS - 1)
    xs = hf[row_tok].reshape(n_blk, EXPERT_BLOCK, D)

    def expert_block(args):
        xb, e = args
        gu = xb @ w1[e] + b1[e]
        g = jnp.minimum(gu[:, :D_EXPERT], SWIGLU_LIMIT)
        u = jnp.clip(gu[:, D_EXPERT:], -SWIGLU_LIMIT, SWIGLU_LIMIT)
        y = (u + 1.0) * g * jax.nn.sigmoid(SWIGLU_ALPHA * g)
        return y @ w2[e] + b2[e]

    ys = lax.map(expert_block, (xs, blk_expert)).reshape(m_pad, D)
    out = jnp.zeros((n_tok, D), jnp.float32).at[row_tok].add(ys.astype(jnp.float32) * row_gate[:, None])
    return out.reshape(B, S, D).astype(h.dtype)


def setup_inputs(seed: int = 0) -> dict:
    key = jax.random.key(seed)
    ks = jax.random.split(key, 27)
    nrm = lambda k, shape, s: s * jax.random.normal(k, shape, jnp.float32)
    gain = lambda k, shape: 1.0 + 0.02 * jax.random.normal(k, shape, jnp.float32)
    L, D, E, F, Hm = DEPTH, D_MODEL, N_EXPERTS, D_EXPERT, HEADS_PER_MIXER
    return {
        'x': nrm(ks[0], (BATCH, SEQ, D), 1.0),
        'c': nrm(ks[1], (BATCH, D), 1.0),
        'w_mod': nrm(ks[2], (L, D, 6 * D), 0.5 * D ** -0.5),
        'b_mod': nrm(ks[3], (L, 6 * D), 0.01),
        'norm1_g': gain(ks[4], (L, D)),
        'norm2_g': gain(ks[5], (L, D)),
        'w_in': nrm(ks[6], (L, D, IN_COLS), D ** -0.5),
        'mla_cq_g': gain(ks[7], (L, MLA_Q_RANK)),
        'mla_w_uq': nrm(ks[8], (L, MLA_Q_RANK, Hm * (MLA_NOPE + MLA_ROPE)), MLA_Q_RANK ** -0.5),
        'mla_ckv_g': gain(ks[9], (L, MLA_KV_RANK)),
        'mla_w_ukv': nrm(ks[10], (L, MLA_KV_RANK, Hm * (MLA_NOPE + MLA_V)), MLA_KV_RANK ** -0.5),
        'mla_q_g': gain(ks[11], (L, MLA_NOPE + MLA_ROPE)),
        'mla_k_g': gain(ks[12], (L, MLA_NOPE + MLA_ROPE)),
        'fox_q_g': gain(ks[13], (L, HEAD_DIM)),
        'fox_k_g': gain(ks[14], (L, HEAD_DIM)),
        'fox_b_f': nrm(ks[15], (L, Hm), 0.1),
        'moba_q_g': gain(ks[16], (L, HEAD_DIM)),
        'moba_k_g': gain(ks[17], (L, HEAD_DIM)),
        'dil_q_g': gain(ks[18], (L, HEAD_DIM)),
        'dil_k_g': gain(ks[19], (L, HEAD_DIM)),
        'w_out': nrm(ks[20], (L, MIX_WIDTH, D), MIX_WIDTH ** -0.5),
        'router_w': nrm(ks[21], (L, D, E), D ** -0.5),
        'router_b': nrm(ks[22], (L, E), 0.01),
        'exp_w1': nrm(ks[23], (L, E, D, 2 * F), D ** -0.5),
        'exp_b1': nrm(ks[24], (L, E, 2 * F), 0.01),
        'exp_w2': nrm(ks[25], (L, E, F, D), F ** -0.5),
        'exp_b2': nrm(ks[26], (L, E, D), 0.01),
    }


def reference(x, c, w_mod, b_mod, norm1_g, norm2_g, w_in, mla_cq_g, mla_w_uq, mla_ckv_g, mla_w_ukv,
              mla_q_g, mla_k_g, fox_q_g, fox_k_g, fox_b_f, moba_q_g, moba_k_g, dil_q_g, dil_k_g, w_out,
              router_w, router_b, exp_w1, exp_b1, exp_w2, exp_b2):
    for l in range(DEPTH):
        mod = jax.nn.silu(c) @ w_mod[l] + b_mod[l]
        sh1, sc1, g1, sh2, sc2, g2 = jnp.split(mod[:, None, :], 6, axis=-1)
        h = rms_norm(x, norm1_g[l]) * (1 + sc1) + sh1
        x = x + g1 * token_mixers(h, w_in[l], mla_cq_g[l], mla_w_uq[l], mla_ckv_g[l], mla_w_ukv[l],
                                  mla_q_g[l], mla_k_g[l], fox_q_g[l], fox_k_g[l], fox_b_f[l],
                                  moba_q_g[l], moba_k_g[l], dil_q_g[l], dil_k_g[l], w_out[l])
        h = rms_norm(x, norm2_g[l]) * (1 + sc2) + sh2
        x = x + g2 * moe_ffn(h, router_w[l], router_b[l], exp_w1[l], exp_b1[l], exp_w2[l], exp_b2[l])
    return x
```

```python
import ml_dtypes
from contextlib import ExitStack
import numpy as np
import concourse.bass as bass
import concourse.mybir as mybir
from concourse.bass_utils import run_bass_kernel_spmd

F32 = mybir.dt.float32
BF16 = mybir.dt.bfloat16
I32 = mybir.dt.int32
AF = mybir.ActivationFunctionType
ALU = mybir.AluOpType
AX = mybir.AxisListType


class Res:
    __slots__ = ("name", "w", "r")

    def __init__(self, name=""):
        self.name = name
        self.w = None
        self.r = []


class Prog:
    NPOOL = 24

    def __init__(self, nc):
        self.nc = nc
        self.eng = {"pe": nc.tensor, "act": nc.scalar, "dve": nc.vector,
                    "pool": nc.gpsimd, "sp": nc.sync}
        self.sems = {}
        self._ctx = []
        for e in self.eng:
            cm = nc.semaphore("s_" + e)
            self.sems[e] = cm.__enter__()
            self._ctx.append(cm)
        self.cnt = {e: 0 for e in self.eng}
        self.dpool = []
        for i in range(self.NPOOL):
            cm = nc.semaphore("d%d" % i)
            self.dpool.append(cm.__enter__())
            self._ctx.append(cm)
        self.dcnt = 0
        self.waited = {e: {} for e in self.eng}
        self.nwaits = 0
        self.nops = 0

    def _sem(self, key):
        if isinstance(key, str):
            return self.sems[key]
        return self.dpool[key]

    def _wait(self, e, tok):
        key, val = tok
        if key == e and e == "pe":
            return
        if key == e and self.cnt[e] < val:
            return
        w = self.waited[e]
        if w.get(key, 0) >= val:
            return
        w[key] = val
        self.eng[e].wait_ge(self._sem(key), val)
        self.nwaits += 1

    def _deps(self, e, reads, writes):
        toks = {}
        for r in reads:
            if r.w is not None:
                k, v = r.w
                if toks.get(k, 0) < v:
                    toks[k] = v
        for wv in writes:
            if wv.w is not None:
                k, v = wv.w
                if toks.get(k, 0) < v:
                    toks[k] = v
            for (k, v) in wv.r:
                if toks.get(k, 0) < v:
                    toks[k] = v
        for k, v in toks.items():
            self._wait(e, (k, v))

    def _mark(self, tok, reads, writes):
        for r in reads:
            r.r.append(tok)
            if len(r.r) > 64:
                d = {}
                for k, v in r.r:
                    if d.get(k, 0) < v:
                        d[k] = v
                r.r = list(d.items())
        for wv in writes:
            wv.w = tok
            wv.r = []

    def op(self, e, fn, reads=(), writes=()):
        self._deps(e, reads, writes)
        ins = fn(self.eng[e])
        self.cnt[e] += 1
        ins.then_inc(self.sems[e], 1)
        tok = (e, self.cnt[e])
        self._mark(tok, reads, writes)
        self.nops += 1
        return tok

    def dma(self, q, out, in_, reads=(), writes=(), **kw):
        slot = self.dcnt % self.NPOOL
        gen = self.dcnt // self.NPOOL
        self.dcnt += 1
        if gen > 0:
            self._wait(q, (slot, 16 * gen))
        self._deps(q, reads, writes)
        ins = self.eng[q].dma_start(out=out, in_=in_, **kw)
        ins.then_inc(self.dpool[slot], 16)
        tok = (slot, 16 * (gen + 1))
        self._mark(tok, reads, writes)
        self.nops += 1
        return tok

    def barrier(self):
        toks = [(e, self.cnt[e]) for e in self.eng if self.cnt[e] > 0]
        for i in range(min(self.dcnt, self.NPOOL)):
            last = self.dcnt - 1 - ((self.dcnt - 1 - i) % self.NPOOL)
            toks.append((i, 16 * (last // self.NPOOL + 1)))
        for e in self.eng:
            for t in toks:
                self._wait(e, t)

    def finish(self):
        self.barrier()

    def close(self):
        for cm in reversed(self._ctx):
            cm.__exit__(None, None, None)


NCORE = 8
NB = 16
SEQ = 16384
D = 1024
NTOK = NB * 128
EPS = 1e-6
BFNP = ml_dtypes.bfloat16
COLS = dict(cq=(0, 256), ckv=(256, 384), kr=(384, 416), fq=(416, 672), fk=(672, 928), fv=(928, 1184),
            flog=(1184, 1188), mq=(1188, 1444), mk=(1444, 1700), mv=(1700, 1956), dq=(1956, 2212),
            dk=(2212, 2468), dv=(2468, 2724))
VOFF = dict(norm1=(0, 1024), cq_g=(1024, 1280), ckv_g=(1280, 1408), q_g=(1408, 1504), k_g=(1504, 1600),
            fq_g=(1600, 1664), fk_g=(1664, 1728), mq_g=(1728, 1792), mk_g=(1792, 1856),
            dq_g=(1856, 1920), dk_g=(1920, 1984), fb=(1984, 1988))


class T:
    def __init__(self, t, name):
        self.t = t
        self.r = Res(name)

    def __getitem__(self, k):
        return self.t[k]


class Ctx:
    def __init__(self):
        self.nc = bass.Bass("TRN2", target_bir_lowering=False)
        self.es = ExitStack()
        self.P = Prog(self.nc)
        self.n = 0
        self.scopes = []

    def dram(self, name, shape, dtype, kind):
        t = T(self.nc.dram_tensor(name, list(shape), dtype, kind=kind).ap(), name)
        return t

    def sb(self, shape, dtype, name=None):
        self.n += 1
        name = name or ("t%d" % self.n)
        es = self.scopes[-1] if self.scopes else self.es
        return T(es.enter_context(self.nc.sbuf_tensor(name, list(shape), dtype)), name)

    def push(self):
        self.scopes.append(ExitStack())

    def pop(self):
        self.P.barrier()
        self.scopes.pop().close()

    def ps(self, shape, dtype, name=None):
        self.n += 1
        name = name or ("p%d" % self.n)
        full = [128, 512] if dtype == F32 else [128, 1024]
        t = T(self.es.enter_context(self.nc.psum_tensor(name, full, dtype)), name)
        t.excl = True
        return t

    def op(self, e, fn, reads=(), writes=()):
        rr = [t.r for t in reads if not getattr(t, "excl", False)]
        ww = [t.r for t in writes] + [t.r for t in reads if getattr(t, "excl", False)]
        return self.P.op(e, fn, rr, ww)

    def dma(self, q, out, in_, reads=(), writes=(), **kw):
        return self.P.dma(q, out, in_, [t.r for t in reads], [t.r for t in writes], **kw)

    def done(self):
        self.P.finish()
        self.es.close()
        self.P.close()
        return self.nc


def make_ident(C):
    idf = C.sb([128, 128], F32)
    idb = C.sb([128, 128], BF16)
    C.op("pool", lambda e: e.iota(idf[:], [[1, 128]], base=0, channel_multiplier=-1,
                                  allow_small_or_imprecise_dtypes=True), writes=[idf])
    C.op("dve", lambda e: e.tensor_single_scalar(idb[:], idf[:], 0.0, ALU.is_equal), reads=[idf], writes=[idb])
    idf2 = C.sb([128, 128], F32)
    C.op("dve", lambda e: e.tensor_single_scalar(idf2[:], idf[:], 0.0, ALU.is_equal), reads=[idf], writes=[idf2])
    return idb, idf2


def emit_mod(C, cT, wmod, bmod, ncols, mps):
    mod = C.sb([128, ncols], F32)
    C.push()
    cs = C.sb([128, 8], F32)
    rep = C.sb([128, 8, 128], F32)
    ones1 = C.sb([1, 128], F32)
    brow = C.sb([1, ncols], F32)
    C.dma("sp", cs[:], cT[:], writes=[cs])
    C.dma("sp", brow[:], bmod[:], writes=[brow])
    C.op("act", lambda e: e.activation(cs[:], cs[:], AF.Silu), reads=[cs], writes=[cs])
    C.op("dve", lambda e: e.memset(ones1[:], 1.0), writes=[ones1])
    for kc in range(8):
        C.op("dve", lambda e, kc=kc: e.tensor_copy(rep[:, kc, :], cs[:, kc:kc + 1].to_broadcast([128, 128])),
             reads=[cs], writes=[rep])
    wst = [C.sb([128, 8, 512], F32) for _ in range(2)]
    for ch in range(ncols // 512):
        w = wst[ch % 2]
        C.dma("sp", w[:], wmod[:, ch * 512:(ch + 1) * 512].rearrange("(kc p) n -> p kc n", p=128), writes=[w])
        for kc in range(8):
            C.op("pe", lambda e, kc=kc, w=w: e.matmul(mps[:], rep[:, kc, :], w[:, kc, :], start=(kc == 0), stop=False),
                 reads=[rep, w], writes=[mps])
        C.op("pe", lambda e, ch=ch: e.matmul(mps[:], ones1[:], brow[:, ch * 512:(ch + 1) * 512], start=False, stop=True),
             reads=[ones1, brow], writes=[mps])
        C.op("act", lambda e, ch=ch: e.copy(mod[:, ch * 512:(ch + 1) * 512], mps[:]), reads=[mps], writes=[mod])
    C.pop()
    return mod


def rstd_of(C, ss, n, dim, tmp, out):
    C.op("act", lambda e: e.activation(tmp[:, 0:n], ss[:, 0:n], AF.Sqrt, bias=EPS, scale=1.0 / dim),
         reads=[ss], writes=[tmp])
    C.op("dve", lambda e: e.reciprocal(out[:, 0:n], tmp[:, 0:n]), reads=[tmp], writes=[out])


def build_pre():
    C = Ctx()
    nc = C.nc
    x = C.dram("x", [NB, 128, D], F32, "ExternalInput")
    cT = C.dram("cT", [128, 8], F32, "ExternalInput")
    wmod = C.dram("wmod", [D, 2048], F32, "ExternalInput")
    bmod = C.dram("bmod", [1, 2048], F32, "ExternalInput")
    vec = C.dram("vec", [1, 2048], F32, "ExternalInput")
    win = C.dram("win", [D, 2724], F32, "ExternalInput")
    wuq = C.dram("wuq", [256, 384], F32, "ExternalInput")
    wukv = C.dram("wukv", [128, 512], F32, "ExternalInput")
    cosd = C.dram("cos", [NB, 128, 16], F32, "ExternalInput")
    sind = C.dram("sin", [NB, 128, 16], F32, "ExternalInput")
    qt = C.dram("qt", [16, 128, NTOK], BF16, "ExternalOutput")
    kt = C.dram("kt", [16, 128, NTOK], BF16, "ExternalOutput")
    vv = C.dram("vv", [16, 128, NB, 65], BF16, "ExternalOutput")
    lf = C.dram("lf", [NB, 128, 4], F32, "ExternalOutput")
    ks = C.dram("ks", [NB, 256], F32, "ExternalOutput")
    qm = C.dram("qm", [NB, 128, 256], F32, "ExternalOutput")

    idb, idf = make_ident(C)
    mps = C.ps([128, 512], F32)
    mod = emit_mod(C, cT, wmod, bmod, 2048, mps)
    vb = C.sb([128, 2048], F32)
    C.dma("sp", vb[:], vec[0].partition_broadcast(128), writes=[vb])
    A1 = C.sb([128, 1024], F32)
    C.op("dve", lambda e: e.scalar_tensor_tensor(A1[:], mod[:, 1024:2048], 1.0, vb[:, 0:1024], ALU.add, ALU.mult),
         reads=[mod, vb], writes=[A1])
    for key, sc in (("q_g", 96 ** -0.5), ("fq_g", 0.125), ("mq_g", 0.125), ("dq_g", 0.125)):
        a, b = VOFF[key]
        C.op("dve", lambda e, a=a, b=b, sc=sc: e.tensor_scalar(vb[:, a:b], vb[:, a:b], sc, None, ALU.mult),
             reads=[vb], writes=[vb])
    winb = C.sb([128, 8, 2724], BF16)
    for kc in range(8):
        for (a, b) in ((0, 1362), (1362, 2724)):
            C.dma("pool", winb[:, kc, a:b], win[kc * 128:(kc + 1) * 128, a:b], writes=[winb])
    wuqb = C.sb([128, 2, 384], BF16)
    C.dma("pool", wuqb[:], wuq[:].rearrange("(kc p) n -> p kc n", p=128), writes=[wuqb])
    wukvb = C.sb([128, 512], BF16)
    C.dma("pool", wukvb[:], wukv[:], writes=[wukvb])
    onesf = C.sb([128, 1], F32)
    C.op("dve", lambda e: e.memset(onesf[:], 1.0), writes=[onesf])

    xt = [C.sb([128, D], F32) for _ in range(2)]
    sq = C.sb([128, D], F32)
    tmpf = C.sb([128, D], F32)
    hb = C.sb([128, D], BF16)
    hT = C.sb([128, 8, 128], BF16)
    proj = C.sb([128, 2724], F32)
    ss = C.sb([128, 16], F32)
    st = C.sb([128, 16], F32)
    rs = C.sb([128, 16], F32)
    cqn = C.sb([128, 256], BF16)
    cqnT = C.sb([128, 2, 128], BF16)
    ckvn = C.sb([128, 128], BF16)
    ckvnT = C.sb([128, 128], BF16)
    qa = C.sb([128, 4, 96], F32)
    ka = C.sb([128, 4, 96], F32)
    qan = C.sb([128, 4, 96], F32)
    kan = C.sb([128, 4, 96], F32)
    rt = [C.sb([128, 16], F32) for _ in range(4)]
    cs_t = C.sb([128, 16], F32)
    sn_t = C.sb([128, 16], F32)
    qab = C.sb([128, 4, 96], BF16)
    kab = C.sb([128, 4, 96], BF16)
    q64 = C.sb([128, 8, 64], BF16)
    k64 = C.sb([128, 12, 64], BF16)
    mkf = C.sb([128, 256], F32)
    qmf = [C.sb([128, 256], F32) for _ in range(2)]
    lft = [C.sb([128, 4], F32) for _ in range(2)]
    l1 = C.sb([128, 4], F32)
    l2 = C.sb([128, 4], F32)
    l3 = C.sb([128, 4], F32)
    kst = [C.sb([1, 256], F32) for _ in range(2)]
    qst = [C.sb([128, 16, 128], BF16) for _ in range(2)]
    kstg = [C.sb([128, 16, 128], BF16) for _ in range(2)]
    vst = [C.sb([128, 16, 65], BF16) for _ in range(2)]
    for t in qst + kstg:
        C.op("pool", lambda e, t=t: e.memset(t[:], 0.0), writes=[t])
    for t in vst:
        C.op("pool", lambda e, t=t: e.memset(t[:], 1.0), writes=[t])
    tp = [C.ps([128, 128], BF16) for _ in range(3)]
    pp = [C.ps([128, 512], F32) for _ in range(2)]
    pq = C.ps([128, 512], F32)
    tpi = [0]
    cpi = [0]

    def transpose_to(dst_fn, src_ap, rows, reads, writes):
        p = tp[tpi[0] % 3]
        tpi[0] += 1
        C.op("pe", lambda e: e.transpose(p[0:rows, 0:128], src_ap, idb[:]), reads=reads + [idb], writes=[p])
        eng = "act" if cpi[0] % 2 == 0 else "dve"
        cpi[0] += 1
        if eng == "act":
            C.op("act", lambda e: e.copy(dst_fn(), p[0:rows, 0:128]), reads=[p], writes=writes)
        else:
            C.op("dve", lambda e: e.tensor_copy(dst_fn(), p[0:rows, 0:128]), reads=[p], writes=writes)

    def sumsq(src_ap, n, dim, width):
        C.op("dve", lambda e: e.tensor_tensor(sq[:, 0:width], src_ap, src_ap, ALU.mult), reads=[proj, qa, ka], writes=[sq])
        C.op("dve", lambda e: e.reduce_sum(ss[:, 0:n], sq[:, 0:width].rearrange("p (h d) -> p h d", h=n), AX.X),
             reads=[sq], writes=[ss])
        rstd_of(C, ss, n, dim, st, rs)

    import os
    NBLK = int(os.environ.get('PRE_NB', NB))
    STAGE = int(os.environ.get('PRE_STAGE', 99))
    for j in range(NBLK):
        X = xt[j % 2]
        C.dma("sp", X[:], x[j], writes=[X])
        C.op("dve", lambda e, X=X: e.tensor_tensor(sq[:], X[:], X[:], ALU.mult), reads=[X], writes=[sq])
        C.op("dve", lambda e: e.reduce_sum(ss[:, 0:1], sq[:], AX.X), reads=[sq], writes=[ss])
        rstd_of(C, ss, 1, D, st, rs)
        C.op("dve", lambda e, X=X: e.scalar_tensor_tensor(tmpf[:], X[:], rs[:, 0:1], A1[:], ALU.mult, ALU.mult),
             reads=[X, rs, A1], writes=[tmpf])
        C.op("pool", lambda e: e.tensor_tensor(hb[:], tmpf[:], mod[:, 0:1024], ALU.add), reads=[tmpf, mod], writes=[hb])
        for kc in range(8):
            transpose_to(lambda kc=kc: hT[:, kc, :], hb[:, kc * 128:(kc + 1) * 128], 128, [hb], [hT])
        for ch in range(6):
            a = ch * 512
            b = min(2724, a + 512)
            p = pp[ch % 2]
            for kc in range(8):
                C.op("pe", lambda e, kc=kc, p=p, a=a, b=b: e.matmul(p[:, 0:b - a], hT[:, kc, :], winb[:, kc, a:b],
                                                                    start=(kc == 0), stop=(kc == 7)),
                     reads=[hT, winb], writes=[p])
            C.op("act", lambda e, p=p, a=a, b=b: e.copy(proj[:, a:b], p[:, 0:b - a]), reads=[p], writes=[proj])
        Q = qst[j % 2]
        K = kstg[j % 2]
        Vs = vst[j % 2]
        if STAGE < 1:
            continue
        sumsq(proj[:, 0:256], 1, 256, 256)
        a, b = VOFF["cq_g"]
        C.op("dve", lambda e, a=a, b=b: e.scalar_tensor_tensor(cqn[:], proj[:, 0:256], rs[:, 0:1], vb[:, a:b], ALU.mult, ALU.mult),
             reads=[proj, rs, vb], writes=[cqn])
        for kc in range(2):
            transpose_to(lambda kc=kc: cqnT[:, kc, :], cqn[:, kc * 128:(kc + 1) * 128], 128, [cqn], [cqnT])
        for kc in range(2):
            C.op("pe", lambda e, kc=kc: e.matmul(pq[:, 0:384], cqnT[:, kc, :], wuqb[:, kc, :], start=(kc == 0), stop=(kc == 1)),
                 reads=[cqnT, wuqb], writes=[pq])
        C.op("act", lambda e: e.copy(qa[:].rearrange("p h d -> p (h d)"), pq[:, 0:384]), reads=[pq], writes=[qa])
        sumsq(proj[:, 256:384], 1, 128, 128)
        a, b = VOFF["ckv_g"]
        C.op("dve", lambda e, a=a, b=b: e.scalar_tensor_tensor(ckvn[:], proj[:, 256:384], rs[:, 0:1], vb[:, a:b], ALU.mult, ALU.mult),
             reads=[proj, rs, vb], writes=[ckvn])
        transpose_to(lambda: ckvnT[:], ckvn[:], 128, [ckvn], [ckvnT])
        C.op("pe", lambda e: e.matmul(pq[:], ckvnT[:], wukvb[:], start=True, stop=True), reads=[ckvnT, wukvb], writes=[pq])
        pq3 = pq[:].rearrange("p (h d) -> p h d", h=4)
        C.op("act", lambda e: e.copy(ka[:, :, 0:64], pq3[:, :, 0:64]), reads=[pq], writes=[ka])
        C.op("dve", lambda e, Vs=Vs: e.tensor_copy(Vs[:, 0:4, 0:64], pq3[:, :, 64:128]), reads=[pq], writes=[Vs])
        for h in range(4):
            C.op("pool", lambda e, h=h: e.tensor_copy(ka[:, h, 64:96], proj[:, 384:416]), reads=[proj], writes=[ka])
        C.dma("sp", cs_t[:], cosd[j], writes=[cs_t])
        C.dma("sp", sn_t[:], sind[j], writes=[sn_t])
        for (src, dstn, dstb, gkey) in ((qa, qan, qab, "q_g"), (ka, kan, kab, "k_g")):
            sumsq(src[:].rearrange("p h d -> p (h d)"), 4, 96, 384)
            ga, gb = VOFF[gkey]
            for h in range(4):
                C.op("dve", lambda e, h=h, src=src, dstn=dstn, ga=ga, gb=gb: e.scalar_tensor_tensor(
                    dstn[:, h, :], src[:, h, :], rs[:, h:h + 1], vb[:, ga:gb], ALU.mult, ALU.mult),
                    reads=[src, rs, vb], writes=[dstn])
            C.op("act", lambda e, dstn=dstn, dstb=dstb: e.copy(dstb[:, :, 0:64], dstn[:, :, 0:64]), reads=[dstn], writes=[dstb])
            for h in range(4):
                x1 = dstn[:, h, 64:80]
                x2 = dstn[:, h, 80:96]
                C.op("dve", lambda e, x1=x1: e.tensor_tensor(rt[0][:], x1, cs_t[:], ALU.mult), reads=[dstn, cs_t], writes=[rt[0]])
                C.op("dve", lambda e, x2=x2: e.tensor_tensor(rt[1][:], x2, sn_t[:], ALU.mult), reads=[dstn, sn_t], writes=[rt[1]])
                C.op("dve", lambda e, h=h, dstb=dstb: e.tensor_tensor(dstb[:, h, 64:80], rt[0][:], rt[1][:], ALU.subtract),
                     reads=[rt[0], rt[1]], writes=[dstb])
                C.op("dve", lambda e, x2=x2: e.tensor_tensor(rt[2][:], x2, cs_t[:], ALU.mult), reads=[dstn, cs_t], writes=[rt[2]])
                C.op("dve", lambda e, x1=x1: e.tensor_tensor(rt[3][:], x1, sn_t[:], ALU.mult), reads=[dstn, sn_t], writes=[rt[3]])
                C.op("dve", lambda e, h=h, dstb=dstb: e.tensor_tensor(dstb[:, h, 80:96], rt[2][:], rt[3][:], ALU.add),
                     reads=[rt[2], rt[3]], writes=[dstb])
        for h in range(4):
            transpose_to(lambda h=h, Q=Q: Q[0:96, h, :], qab[:, h, :], 96, [qab], [Q])
            transpose_to(lambda h=h, K=K: K[0:96, h, :], kab[:, h, :], 96, [kab], [K])
        if STAGE < 2:
            continue
        def norm64(colkey, gkey, dst_fn, dst_t, eng="dve"):
            a, b = COLS[colkey]
            sumsq(proj[:, a:b], 4, 64, 256)
            ga, gb = VOFF[gkey]
            for h in range(4):
                C.op("dve", lambda e, h=h: e.scalar_tensor_tensor(dst_fn(h), proj[:, a + h * 64:a + (h + 1) * 64], rs[:, h:h + 1],
                                                                 vb[:, ga:gb], ALU.mult, ALU.mult),
                     reads=[proj, rs, vb], writes=[dst_t])
        QM = qmf[j % 2]
        norm64("fq", "fq_g", lambda h: q64[:, h, :], q64)
        norm64("fk", "fk_g", lambda h: k64[:, h, :], k64)
        norm64("mq", "mq_g", lambda h: QM[:, h * 64:(h + 1) * 64], QM)
        norm64("mk", "mk_g", lambda h: mkf[:, h * 64:(h + 1) * 64], mkf)
        norm64("dq", "dq_g", lambda h: q64[:, 4 + h, :], q64)
        norm64("dk", "dk_g", lambda h: k64[:, 8 + h, :], k64)
        C.op("act", lambda e: e.copy(k64[:, 4:8, :].rearrange("p h d -> p (h d)"), mkf[:]), reads=[mkf], writes=[k64])
        C.dma("sp", qm[j], QM[:], reads=[QM], writes=[qm])
        KS = kst[j % 2]
        C.op("pe", lambda e: e.matmul(pq[0:1, 0:256], onesf[:], mkf[:], start=True, stop=True), reads=[onesf, mkf], writes=[pq])
        C.op("act", lambda e, KS=KS: e.copy(KS[:], pq[0:1, 0:256]), reads=[pq], writes=[KS])
        C.dma("sp", ks[j:j + 1, :], KS[:], reads=[KS], writes=[ks])
        for gi, key in enumerate(("fv", "mv", "dv")):
            a, b = COLS[key]
            C.op("pool", lambda e, gi=gi, a=a, b=b, Vs=Vs: e.tensor_copy(
                Vs[:, 4 + 4 * gi:8 + 4 * gi, 0:64], proj[:, a:b].rearrange("p (h d) -> p h d", h=4)), reads=[proj], writes=[Vs])
        a, b = COLS["flog"]
        fa, fb = VOFF["fb"]
        LF = lft[j % 2]
        C.op("dve", lambda e: e.tensor_tensor(l1[:], proj[:, a:b], vb[:, fa:fb], ALU.add), reads=[proj, vb], writes=[l1])
        C.op("dve", lambda e: e.tensor_scalar(l3[:], l1[:], -1.0, None, ALU.mult), reads=[l1], writes=[l3])
        C.op("dve", lambda e: e.tensor_tensor(l2[:], l3[:], l1[:], ALU.max), reads=[l1, l3], writes=[l2])
        C.op("act", lambda e: e.activation(l2[:], l2[:], AF.Exp, scale=-1.0), reads=[l2], writes=[l2])
        C.op("act", lambda e: e.activation(l2[:], l2[:], AF.Ln, bias=1.0), reads=[l2], writes=[l2])
        C.op("dve", lambda e: e.tensor_single_scalar(l3[:], l1[:], 0.0, ALU.min), reads=[l1], writes=[l3])
        C.op("dve", lambda e, LF=LF: e.tensor_tensor(LF[:], l3[:], l2[:], ALU.subtract), reads=[l3, l2], writes=[LF])
        C.dma("sp", lf[j], LF[:], reads=[LF], writes=[lf])
        for (srcT, s0, dstT, h0) in ((q64, 0, Q, 4), (q64, 4, Q, 12), (k64, 0, K, 4), (k64, 4, K, 8), (k64, 8, K, 12)):
            for hh in range(4):
                transpose_to(lambda dstT=dstT, h0=h0, hh=hh: dstT[0:64, h0 + hh, :], srcT[:, s0 + hh, :], 64, [srcT], [dstT])
        if STAGE < 3:
            continue
        C.dma("sp", qt[:].rearrange("h p t -> p h t")[:, :, j * 128:(j + 1) * 128], Q[:], reads=[Q], writes=[qt])
        C.dma("sp", kt[:].rearrange("h p t -> p h t")[:, :, j * 128:(j + 1) * 128], K[:], reads=[K], writes=[kt])
        C.dma("sp", vv[:].rearrange("h k j e -> k h j e")[:, :, j, :], Vs[:], reads=[Vs], writes=[vv])
    return C.done()


def own_blocks(a, c):
    return np.ascontiguousarray(a.reshape(NB, NCORE, 128, *a.shape[1:])[:, c])


def rope_tables():
    half = 16
    inv = (np.float32(1.0) / np.power(np.float32(10000.0), np.arange(half, dtype=np.float32) / np.float32(half))).astype(np.float32)
    pos = np.arange(SEQ, dtype=np.float32)
    ang = (pos[:, None] * inv[None, :]).astype(np.float32)
    return np.cos(ang.astype(np.float64)).astype(np.float32), np.sin(ang.astype(np.float64)).astype(np.float32)


def pack_vec(inp, l):
    v = np.zeros((1, 2048), np.float32)
    for key, name in (("norm1", "norm1_g"), ("cq_g", "mla_cq_g"), ("ckv_g", "mla_ckv_g"), ("q_g", "mla_q_g"),
                      ("k_g", "mla_k_g"), ("fq_g", "fox_q_g"), ("fk_g", "fox_k_g"), ("mq_g", "moba_q_g"),
                      ("mk_g", "moba_k_g"), ("dq_g", "dil_q_g"), ("dk_g", "dil_k_g"), ("fb", "fox_b_f")):
        a, b = VOFF[key]
        v[0, a:b] = inp[name][l]
    return v


def pre_inputs(inp, l, xcur):
    cos, sin = rope_tables()
    cT = np.ascontiguousarray(inp["c"].reshape(8, 128).T)
    maps = []
    for c in range(NCORE):
        maps.append(dict(
            x=own_blocks(xcur, c), cT=cT,
            wmod=np.ascontiguousarray(inp["w_mod"][l][:, 0:2048]),
            bmod=np.ascontiguousarray(inp["b_mod"][l][None, 0:2048]),
            vec=pack_vec(inp, l), win=inp["w_in"][l], wuq=inp["mla_w_uq"][l], wukv=inp["mla_w_ukv"][l],
            cos=own_blocks(cos, c), sin=own_blocks(sin, c)))
    return maps


def build_post():
    C = Ctx()
    x = C.dram("x", [NB, 128, D], F32, "ExternalInput")
    ot = C.dram("ot", [D, NTOK], BF16, "ExternalInput")
    cT = C.dram("cT", [128, 8], F32, "ExternalInput")
    wmod = C.dram("wmod", [D, 4096], F32, "ExternalInput")
    bmod = C.dram("bmod", [1, 4096], F32, "ExternalInput")
    vec = C.dram("vec", [1, 1152], F32, "ExternalInput")
    wout = C.dram("wout", [D, D], F32, "ExternalInput")
    rw = C.dram("rw", [D, 32], F32, "ExternalInput")
    w1 = C.dram("w1", [32, D, 2048], F32, "ExternalInput")
    b1 = C.dram("b1", [32, 2048], F32, "ExternalInput")
    w2 = C.dram("w2", [32, D, D], F32, "ExternalInput")
    b2 = C.dram("b2", [32, D], F32, "ExternalInput")
    xo = C.dram("xo", [NB, 128, D], F32, "ExternalOutput")
    x1d = C.dram("x1d", [NB, 128, D], F32, "Internal")

    idb, idf = make_ident(C)
    PS = [C.ps([128, 512], F32) for _ in range(8)]
    mod = emit_mod(C, cT, wmod, bmod, 4096, PS[0])
    vb = C.sb([128, 1152], F32)
    C.dma("sp", vb[:], vec[0].partition_broadcast(128), writes=[vb])
    A2 = C.sb([128, 1024], F32)
    C.op("dve", lambda e: e.scalar_tensor_tensor(A2[:], mod[:, 2048:3072], 1.0, vb[:, 0:1024], ALU.add, ALU.mult),
         reads=[mod, vb], writes=[A2])
    rwf = C.sb([128, 8, 32], F32)
    C.dma("sp", rwf[:], rw[:].rearrange("(kc p) n -> p kc n", p=128), writes=[rwf])
    b2s = C.sb([32, 1024], F32)
    C.dma("sp", b2s[:], b2[:], writes=[b2s])
    b1T = C.sb([128, 16, 32], F32)
    C.push()
    b1s = C.sb([32, 2048], F32)
    C.dma("sp", b1s[:], b1[:], writes=[b1s])
    for ch in range(16):
        p = PS[1 + ch % 2]
        C.op("pe", lambda e, p=p, ch=ch: e.transpose(p[:, 0:32], b1s[:, ch * 128:(ch + 1) * 128], idf[0:32, 0:32]),
             reads=[b1s, idf], writes=[p])
        C.op("act", lambda e, p=p, ch=ch: e.copy(b1T[:, ch, :], p[:, 0:32]), reads=[p], writes=[b1T])
    C.pop()
    HB = NB // 2
    gates = C.sb([128, HB, 32], F32)
    XnT = C.sb([128, 8, HB * 128], BF16)
    Yacc = C.sb([128, HB, D], F32)
    si = [0]

    import os
    NEXP = int(os.environ.get("POST_NEXP", 32))
    for half in range(2):
        C.push()
        woutb = C.sb([128, 8, 1024], BF16)
        for kc in range(8):
            C.dma("pool", woutb[:, kc, :], wout[kc * 128:(kc + 1) * 128, :], writes=[woutb])
        xt = [C.sb([128, D], F32) for _ in range(2)]
        otb = [C.sb([128, 8, 128], BF16) for _ in range(2)]
        x1 = C.sb([128, D], F32)
        sq = C.sb([128, D], F32)
        h2 = C.sb([128, D], F32)
        h2T = C.sb([128, 8, 128], F32)
        ss = C.sb([128, 8], F32)
        st = C.sb([128, 8], F32)
        rs = C.sb([128, 8], F32)
        lg = C.sb([128, 32], F32)
        t8 = C.sb([128, 8], F32)
        nm = C.sb([128, 1], F32)
        mk = C.sb([128, 32], F32)
        ex = C.sb([128, 32], F32)
        den = C.sb([128, 1], F32)
        gT = C.sb([32, 128], F32)
        for jj in range(HB):
            j = half * HB + jj
            X = xt[j % 2]
            O = otb[j % 2]
            C.dma("sp", X[:], x[j], writes=[X])
            C.dma("sp", O[:], ot[:, j * 128:(j + 1) * 128].rearrange("(kc p) t -> p kc t", p=128), writes=[O])
            for n in range(2):
                p = PS[n]
                for kc in range(8):
                    C.op("pe", lambda e, p=p, kc=kc, n=n: e.matmul(p[:], O[:, kc, :], woutb[:, kc, n * 512:(n + 1) * 512],
                                                                   start=(kc == 0), stop=(kc == 7)), reads=[O, woutb], writes=[p])
                C.op("dve", lambda e, p=p, n=n: e.tensor_tensor(sq[:, n * 512:(n + 1) * 512], p[:], mod[:, n * 512:(n + 1) * 512], ALU.mult),
                     reads=[p, mod], writes=[sq])
            C.op("dve", lambda e: e.tensor_tensor(x1[:], sq[:], X[:], ALU.add), reads=[sq, X], writes=[x1])
            C.dma("sp", x1d[j], x1[:], reads=[x1], writes=[x1d])
            C.op("dve", lambda e: e.tensor_tensor(sq[:], x1[:], x1[:], ALU.mult), reads=[x1], writes=[sq])
            C.op("dve", lambda e: e.reduce_sum(ss[:, 0:1], sq[:], AX.X), reads=[sq], writes=[ss])
            rstd_of(C, ss, 1, D, st, rs)
            C.op("dve", lambda e: e.scalar_tensor_tensor(sq[:], x1[:], rs[:, 0:1], A2[:], ALU.mult, ALU.mult),
                 reads=[x1, rs, A2], writes=[sq])
            C.op("pool", lambda e: e.tensor_tensor(h2[:], sq[:], mod[:, 1024:2048], ALU.add), reads=[sq, mod], writes=[h2])
            for kc in range(8):
                p = PS[2 + kc % 2]
                C.op("pe", lambda e, p=p, kc=kc: e.transpose(p[:, 0:128], h2[:, kc * 128:(kc + 1) * 128], idf[:]),
                     reads=[h2, idf], writes=[p])
                C.op("act", lambda e, p=p, kc=kc: e.copy(h2T[:, kc, :], p[:, 0:128]), reads=[p], writes=[h2T])
                C.op("dve", lambda e, p=p, kc=kc, jj=jj: e.tensor_copy(XnT[:, kc, jj * 128:(jj + 1) * 128], p[:, 0:128]),
                     reads=[p], writes=[XnT])
            p = PS[4]
            for kc in range(8):
                C.op("pe", lambda e, p=p, kc=kc: e.matmul(p[:, 0:32], h2T[:, kc, :], rwf[:, kc, :], start=(kc == 0), stop=(kc == 7)),
                     reads=[h2T, rwf], writes=[p])
            C.op("dve", lambda e, p=p: e.tensor_tensor(lg[:], p[:, 0:32], vb[:, 1024:1056], ALU.add), reads=[p, vb], writes=[lg])
            C.op("dve", lambda e: e.max(t8[:], lg[:]), reads=[lg], writes=[t8])
            C.op("dve", lambda e: e.tensor_scalar(mk[:], lg[:], t8[:, 3:4], None, ALU.is_ge), reads=[lg, t8], writes=[mk])
            C.op("dve", lambda e: e.tensor_scalar(nm[:], t8[:, 0:1], -1.0, None, ALU.mult), reads=[t8], writes=[nm])
            C.op("act", lambda e: e.activation(ex[:], lg[:], AF.Exp, bias=nm[:, 0:1], scale=1.0), reads=[lg, nm], writes=[ex])
            C.op("dve", lambda e: e.tensor_tensor(ex[:], ex[:], mk[:], ALU.mult), reads=[ex, mk], writes=[ex])
            C.op("dve", lambda e: e.reduce_sum(den[:], ex[:], AX.X), reads=[ex], writes=[den])
            C.op("dve", lambda e: e.reciprocal(den[:], den[:]), reads=[den], writes=[den])
            C.op("dve", lambda e, jj=jj: e.tensor_scalar(gates[:, jj, :], ex[:], den[:, 0:1], None, ALU.mult),
                 reads=[ex, den], writes=[gates])
            p = PS[5]
            C.op("pe", lambda e, p=p, jj=jj: e.transpose(p[0:32, 0:128], gates[:, jj, :], idf[:]), reads=[gates, idf], writes=[p])
            C.op("act", lambda e, p=p: e.copy(gT[:], p[0:32, 0:128]), reads=[p], writes=[gT])
            for n in range(2):
                p = PS[6 + n]
                C.op("pe", lambda e, p=p, n=n: e.matmul(p[:], gT[:], b2s[:, n * 512:(n + 1) * 512], start=True, stop=True),
                     reads=[gT, b2s], writes=[p])
                C.op("act", lambda e, p=p, n=n, jj=jj: e.copy(Yacc[:, jj, n * 512:(n + 1) * 512], p[:]), reads=[p], writes=[Yacc])
        C.pop()
        C.push()
        actT = C.sb([128, 8, HB * 128], BF16)
        w1b = C.sb([128, 8, 2048], BF16)
        w2b = C.sb([128, 8, 1024], BF16)
        stg = [C.sb([128, 2048], F32) for _ in range(2)]
        gs = [C.sb([128, 512], F32) for _ in range(1)]
        sg = [C.sb([128, 512], F32) for _ in range(1)]
        u1 = [C.sb([128, 512], F32) for _ in range(1)]
        tt_ = [C.sb([128, 512], F32) for _ in range(1)]
        xt = [C.sb([128, D], F32) for _ in range(1)]
        sq = C.sb([128, D], F32)
        for ex_i in range(NEXP):
            for kc in range(8):
                s_ = stg[si[0] % 2]
                si[0] += 1
                C.dma("sp", s_[:], w1[ex_i, kc * 128:(kc + 1) * 128, :], writes=[s_])
                C.op("pool", lambda e, s_=s_, kc=kc: e.tensor_copy(w1b[:, kc, :], s_[:]), reads=[s_], writes=[w1b])
            cnt = 0
            for i in range(8):
                for tt in range(HB * 128 // 512):
                    gp = PS[cnt % 2]
                    up = PS[2 + cnt % 2]
                    G, SG, U, TT = gs[0], sg[0], u1[0], tt_[0]
                    cnt += 1
                    for (pp_, c0) in ((gp, i * 128), (up, 1024 + i * 128)):
                        for kc in range(8):
                            C.op("pe", lambda e, pp_=pp_, c0=c0, kc=kc, tt=tt: e.matmul(
                                pp_[:], w1b[:, kc, c0:c0 + 128], XnT[:, kc, tt * 512:(tt + 1) * 512], start=(kc == 0), stop=(kc == 7)),
                                reads=[w1b, XnT], writes=[pp_])
                    C.op("dve", lambda e, G=G, gp=gp, i=i: e.tensor_scalar(G[:], gp[:], b1T[:, i, ex_i:ex_i + 1], 7.0, ALU.add, ALU.min),
                         reads=[gp, b1T], writes=[G])
                    C.op("act", lambda e, G=G, SG=SG: e.activation(SG[:], G[:], AF.Sigmoid, scale=1.702), reads=[G], writes=[SG])
                    C.op("dve", lambda e, U=U, up=up, i=i: e.tensor_scalar(U[:], up[:], b1T[:, 8 + i, ex_i:ex_i + 1], 7.0, ALU.add, ALU.min),
                         reads=[up, b1T], writes=[U])
                    C.op("dve", lambda e, U=U: e.tensor_scalar(U[:], U[:], -7.0, 1.0, ALU.max, ALU.add), reads=[U], writes=[U])
                    C.op("pool", lambda e, G=G, SG=SG, TT=TT: e.tensor_tensor(TT[:], G[:], SG[:], ALU.mult), reads=[G, SG], writes=[TT])
                    C.op("pool", lambda e, U=U, TT=TT, i=i, tt=tt: e.tensor_tensor(actT[:, i, tt * 512:(tt + 1) * 512], TT[:], U[:], ALU.mult),
                         reads=[TT, U], writes=[actT])
            for kc in range(8):
                s_ = stg[si[0] % 2]
                si[0] += 1
                C.dma("sp", s_[:, 0:1024], w2[ex_i, kc * 128:(kc + 1) * 128, :], writes=[s_])
                C.op("pool", lambda e, s_=s_, kc=kc: e.tensor_copy(w2b[:, kc, :], s_[:, 0:1024]), reads=[s_], writes=[w2b])
            for blk in range(HB):
                for n in range(2):
                    yp = PS[4 + (blk * 2 + n) % 4]
                    for i in range(8):
                        C.op("pe", lambda e, yp=yp, i=i, blk=blk, n=n: e.matmul(
                            yp[:], actT[:, i, blk * 128:(blk + 1) * 128], w2b[:, i, n * 512:(n + 1) * 512], start=(i == 0), stop=(i == 7)),
                            reads=[actT, w2b], writes=[yp])
                    C.op("dve", lambda e, yp=yp, blk=blk, n=n: e.scalar_tensor_tensor(
                        Yacc[:, blk, n * 512:(n + 1) * 512], yp[:], gates[:, blk, ex_i:ex_i + 1], Yacc[:, blk, n * 512:(n + 1) * 512],
                        ALU.mult, ALU.add), reads=[yp, gates, Yacc], writes=[Yacc])
        for jj in range(HB):
            j = half * HB + jj
            X = xt[0]
            C.dma("sp", X[:], x1d[j], reads=[x1d], writes=[X])
            C.op("dve", lambda e, jj=jj: e.tensor_tensor(sq[:], Yacc[:, jj, :], mod[:, 3072:4096], ALU.mult), reads=[Yacc, mod], writes=[sq])
            C.op("dve", lambda e, X=X: e.tensor_tensor(X[:], sq[:], X[:], ALU.add), reads=[sq, X], writes=[X])
            C.dma("sp", xo[j], X[:], reads=[X], writes=[xo])
        C.pop()
    return C.done()


def post_inputs(inp, l, xcur, ot_list):
    cT = np.ascontiguousarray(inp["c"].reshape(8, 128).T)
    vec = np.zeros((1, 1152), np.float32)
    vec[0, 0:1024] = inp["norm2_g"][l]
    vec[0, 1024:1056] = inp["router_b"][l]
    wm = np.ascontiguousarray(inp["w_mod"][l][:, 2048:6144])
    bm = np.ascontiguousarray(inp["b_mod"][l][None, 2048:6144])
    maps = []
    for c in range(NCORE):
        maps.append(dict(x=own_blocks(xcur, c), ot=ot_list[c], cT=cT, wmod=wm, bmod=bm, vec=vec,
                         wout=inp["w_out"][l], rw=inp["router_w"][l], w1=inp["exp_w1"][l], b1=inp["exp_b1"][l],
                         w2=inp["exp_w2"][l], b2=inp["exp_b2"][l]))
    return maps


def rank_of_block(g):
    return (g % 8) * 16 + g // 8


MOBA_SLOPES = [2.0 ** -2, 2.0 ** -4, 2.0 ** -6, 2.0 ** -8]
DIL_SLOPES = [2.0 ** -1, 2.0 ** -3, 2.0 ** -5, 2.0 ** -7]


def build_attn():
    C = Ctx()
    qt = C.dram("qt", [16, 128, NTOK], BF16, "ExternalInput")
    qm = C.dram("qm", [NB, 128, 256], F32, "ExternalInput")
    ktg = C.dram("ktg", [8, 16, 128, NTOK], BF16, "ExternalInput")
    vvg = C.dram("vvg", [8, 16, 128, NB, 65], BF16, "ExternalInput")
    lfg = C.dram("lfg", [8, NB, 128, 4], F32, "ExternalInput")
    ksg = C.dram("ksg", [8, NB, 256], F32, "ExternalInput")
    cmaskd = C.dram("cmask", [128, 8, 128], BF16, "ExternalInput")
    dmaskd = C.dram("dmask", [128, 24, 128], BF16, "ExternalInput")
    Pmd = C.dram("Pm", [128, 64], F32, "ExternalInput")
    Ud = C.dram("U", [128, 128], F32, "ExternalInput")
    selwd = C.dram("selw", [128, 16, 69], BF16, "ExternalInput")
    pastd = C.dram("pastneg", [1, 1024], F32, "ExternalInput")
    aseld = C.dram("asel", [1, 1024], F32, "ExternalInput")
    bcd = C.dram("bc", [4, 1024], F32, "ExternalInput")
    dilqd = C.dram("dilq", [4, 2, NTOK], BF16, "ExternalInput")
    kbsd = C.dram("kbs", [8, 128, 128], F32, "ExternalInput")
    ot = C.dram("ot", [D, NTOK], BF16, "ExternalOutput")

    idb, idf = make_ident(C)
    PS = [C.ps([128, 512], F32) for _ in range(7)]
    PB = C.ps([128, 128], BF16)
    SR, OP_, BCP, M0, M1 = PS[0:3], PS[3:5], PS[5], PS[6], PS[6]
    kbuf = [C.sb([128, SEQ], BF16) for _ in range(2)]
    vbuf = [C.sb([128, 128, 65], BF16) for _ in range(2)]
    qbuf = [C.sb([128, NTOK], BF16) for _ in range(2)]
    ostg = [C.sb([64, NTOK], BF16) for _ in range(2)]
    cmask = C.sb([128, 8, 128], BF16)
    dmask = C.sb([128, 24, 128], BF16)
    Pm = C.sb([128, 64], F32)
    U = C.sb([128, 128], F32)
    selw = C.sb([128, 16, 69], BF16)
    past = C.sb([128, 16, 64], F32)
    asel = C.sb([128, 16, 64], F32)
    bch = C.sb([128, 16, 64], F32)
    kbs = C.sb([128, 8, 128], F32)
    kbF = C.sb([128, 4, 128], F32)
    LF = C.sb([128, 128, 4], F32)
    qms = C.sb([128, NB, 256], F32)
    kss = C.sb([128, 256], F32)
    onesf = C.sb([128, 128], F32)
    zcol = C.sb([128, 1], F32)
    C.op("dve", lambda e: e.memset(onesf[:], 1.0), writes=[onesf])
    C.op("dve", lambda e: e.memset(zcol[:], 0.0), writes=[zcol])
    C.dma("sp", cmask[:], cmaskd[:], writes=[cmask])
    C.dma("sp", dmask[:], dmaskd[:], writes=[dmask])
    C.dma("sp", Pm[:], Pmd[:], writes=[Pm])
    C.dma("sp", U[:], Ud[:], writes=[U])
    C.dma("sp", selw[:], selwd[:], writes=[selw])
    C.dma("sp", past[:].rearrange("p j n -> p (j n)"), pastd[0].partition_broadcast(128), writes=[past])
    C.dma("sp", asel[:].rearrange("p j n -> p (j n)"), aseld[0].partition_broadcast(128), writes=[asel])
    C.dma("sp", kbs[:], kbsd[:].rearrange("h k p -> k h p"), writes=[kbs])
    C.dma("sp", LF[:], lfg[:].rearrange("r j k h -> (r j) k h"), writes=[LF])
    C.dma("sp", qms[:], qm[:].rearrange("j k f -> k j f"), writes=[qms])
    C.dma("sp", kss[:], ksg[:].rearrange("r j f -> (r j) f"), writes=[kss])

    rowcs = C.sb([128, 4, 128], F32)
    tot = C.sb([128, 4], F32)
    off = C.sb([128, 4], F32)
    Frow = C.sb([128, 128], F32)
    r1 = C.sb([128, 128], F32)
    Fp = C.sb([128, 4, 3, 128], BF16)
    for hf in range(4):
        C.op("dve", lambda e, hf=hf: e.tensor_tensor_scan(rowcs[:, hf, :], onesf[:], LF[:, :, hf], 0.0, ALU.mult, ALU.add),
             reads=[onesf, LF], writes=[rowcs])
        C.op("dve", lambda e, hf=hf: e.tensor_copy(tot[:, hf:hf + 1], rowcs[:, hf, 127:128]), reads=[rowcs], writes=[tot])
    C.op("pe", lambda e: e.matmul(M0[:, 0:4], U[:], tot[:], start=True, stop=True), reads=[U, tot], writes=[M0])
    C.op("act", lambda e: e.copy(off[:], M0[:, 0:4]), reads=[M0], writes=[off])
    for hf in range(4):
        C.op("dve", lambda e, hf=hf: e.tensor_scalar(Frow[:], rowcs[:, hf, :], off[:, hf:hf + 1], None, ALU.add),
             reads=[rowcs, off], writes=[Frow])
        C.op("pe", lambda e: e.transpose(M0[:, 0:128], Frow[:], idf[:]), reads=[Frow, idf], writes=[M0])
        C.op("act", lambda e, hf=hf: e.activation(kbF[:, hf, :], M0[:, 0:128], AF.Copy, scale=-1.0), reads=[M0], writes=[kbF])
        C.op("dve", lambda e, hf=hf: e.tensor_copy(Fp[:, hf, 0, :], Frow[:]), reads=[Frow], writes=[Fp])
        C.op("dve", lambda e, hf=hf: e.tensor_tensor(r1[:], Frow[:], Fp[:, hf, 0, :], ALU.subtract), reads=[Frow, Fp], writes=[r1])
        C.op("dve", lambda e, hf=hf: e.tensor_copy(Fp[:, hf, 1, :], r1[:]), reads=[r1], writes=[Fp])
        C.op("dve", lambda e, hf=hf: e.tensor_tensor(r1[:], r1[:], Fp[:, hf, 1, :], ALU.subtract), reads=[r1, Fp], writes=[r1])
        C.op("dve", lambda e, hf=hf: e.tensor_copy(Fp[:, hf, 2, :], r1[:]), reads=[r1], writes=[Fp])

    pT = [C.sb([128, 128], BF16) for _ in range(4)]
    osb = C.sb([128, 128], F32)
    rec = C.sb([128, 128], F32)
    kmT = C.sb([64, 64], F32)
    qT32 = C.sb([64, 128], F32)
    gm = C.sb([128, 64], F32)
    t8 = C.sb([128, 8], F32)
    sel = C.sb([128, 64], F32)
    aug = C.sb([128, 128], BF16)
    tmpi = C.sb([128, 2048], F32)
    import os
    HEADS = [int(v) for v in os.environ.get("ATT_HEADS", ",".join(str(i) for i in range(16))).split(",")]
    NQ = int(os.environ.get("ATT_NQ", NB))
    tile_i = [0]
    for hi, h in enumerate(HEADS):
        typ = h // 4
        hh = h % 4
        slot = hi % 2
        KB, VB, QB, OS = kbuf[slot], vbuf[slot], qbuf[slot], ostg[slot]
        r0 = 96 if typ == 0 else 64
        R = (96, 67, 128, 66)[typ]
        for r in range(8):
            C.dma("sp", KB[0:r0, r * NTOK:(r + 1) * NTOK], ktg[r, h, 0:r0, :], writes=[KB])
        C.dma("sp", VB[:].rearrange("k (r j) e -> k r (j e)", r=8), vvg[:, h].rearrange("r k j e -> k r (j e)"), writes=[VB])
        if typ != 2:
            C.dma("sp", QB[0:r0, :], qt[h, 0:r0, :], writes=[QB])
        if typ == 1:
            C.op("pool", lambda e: e.memset(KB[64:67, :], 1.0), writes=[KB])
        if typ == 3:
            C.op("pool", lambda e: e.memset(KB[64:66, :], 1.0), writes=[KB])
            C.dma("sp", QB[64:66, :], dilqd[hh], writes=[QB])
        if typ == 2:
            C.dma("sp", bch[:].rearrange("p j n -> p (j n)"), bcd[hh].partition_broadcast(128), writes=[bch])
            C.op("pe", lambda e: e.matmul(M0[0:64, 0:64], kss[:, hh * 64:(hh + 1) * 64], Pm[:], start=True, stop=True),
                 reads=[kss, Pm], writes=[M0])
            C.op("act", lambda e: e.copy(kmT[:], M0[0:64, 0:64]), reads=[M0], writes=[kmT])
            for r in range(8):
                C.op("pool", lambda e, r=r: e.iota(tmpi[64:128, :], [[4, 16], [0, 128]], base=r // 2, channel_multiplier=-1,
                                                   allow_small_or_imprecise_dtypes=True), writes=[tmpi])
                C.op("dve", lambda e, r=r: e.tensor_single_scalar(KB[64:128, r * NTOK:(r + 1) * NTOK], tmpi[64:128, :], 0.0, ALU.is_equal),
                     reads=[tmpi], writes=[KB])
        for j in range(NQ):
            qs = slice(j * 128, (j + 1) * 128)
            if typ == 1:
                for i in range(3):
                    C.op("pe", lambda e, i=i: e.matmul(M0[0:67, 0:128], selw[:, j, 2 - i:2 - i + 67], Fp[:, hh, i, :],
                                                       start=(i == 0), stop=(i == 2)), reads=[selw, Fp], writes=[M0])
                C.op("act", lambda e: e.copy(QB[64:67, qs], M0[64:67, 0:128]), reads=[M0], writes=[QB])
            if typ == 2:
                qf = qms[:, j, hh * 64:(hh + 1) * 64]
                C.op("pe", lambda e: e.transpose(M0[0:64, 0:128], qf, idf[:]), reads=[qms, idf], writes=[M0])
                C.op("act", lambda e: e.copy(qT32[:], M0[0:64, 0:128]), reads=[M0], writes=[qT32])
                C.op("pe", lambda e: e.matmul(M0[:, 0:64], qT32[:], kmT[:], start=True, stop=True), reads=[qT32, kmT], writes=[M0])
                C.op("dve", lambda e: e.tensor_tensor(gm[:], M0[:, 0:64], past[:, j, :], ALU.add), reads=[M0, past], writes=[gm])
                C.op("dve", lambda e: e.max(t8[:], gm[:]), reads=[gm], writes=[t8])
                C.op("dve", lambda e: e.tensor_scalar(sel[:], gm[:], t8[:, 2:3], None, ALU.is_ge), reads=[gm, t8], writes=[sel])
                C.op("dve", lambda e: e.tensor_tensor(sel[:], sel[:], asel[:, j, :], ALU.mult), reads=[sel, asel], writes=[sel])
                C.op("dve", lambda e: e.tensor_tensor(aug[:, 64:128], sel[:], bch[:, j, :], ALU.add), reads=[sel, bch], writes=[aug])
                C.op("act", lambda e: e.copy(aug[:, 0:64], qf), reads=[qms], writes=[aug])
                C.op("pe", lambda e: e.transpose(PB[:, 0:128], aug[:], idb[:]), reads=[aug, idb], writes=[PB])
                C.op("act", lambda e: e.copy(QB[:, qs], PB[:, 0:128]), reads=[PB], writes=[QB])
            if typ == 3:
                glist = [(g, g - (8 * j - 16)) for g in range(max(0, 8 * j - 16), 8 * j + 8)]
            else:
                glist = [(g, g - 8 * j) for g in range(0, 8 * j + 8)]
            OPS = OP_[(hi * NB + j) % 2]
            n = len(glist)
            LA = 2
            slots = []
            for idx in range(n + LA):
                if idx < n:
                    g, i = glist[idx]
                    p = rank_of_block(g)
                    sp_ = SR[tile_i[0] % 3]
                    pt_ = pT[tile_i[0] % 4]
                    tile_i[0] += 1
                    slots.append((p, pt_))
                    masked = (typ != 3 and i >= 0) or (typ == 3 and i >= 16)
                    mi = i - 16 if typ == 3 else i
                    C.op("pe", lambda e, p=p, sp_=sp_: e.matmul(sp_[:, 0:128], KB[0:R, p * 128:(p + 1) * 128], QB[0:R, qs],
                                                                start=True, stop=not masked), reads=[KB, QB], writes=[sp_])
                    if masked:
                        C.op("pe", lambda e, mi=mi, sp_=sp_: e.matmul(sp_[:, 0:128], idb[:], cmask[:, mi, :], start=False, stop=True),
                             reads=[idb, cmask], writes=[sp_])
                    if typ == 0:
                        bias_ap, bt = zcol[:, 0:1], zcol
                    elif typ == 1:
                        bias_ap, bt = kbF[:, hh, p:p + 1], kbF
                    else:
                        hb_ = hh if typ == 2 else 4 + hh
                        bias_ap, bt = kbs[:, hb_, p:p + 1], kbs
                    C.op("act", lambda e, sp_=sp_, pt_=pt_, bias_ap=bias_ap: e.activation(pt_[:], sp_[:, 0:128], AF.Exp, bias=bias_ap, scale=1.0),
                         reads=[sp_, bt], writes=[pt_])
                    if typ == 3:
                        C.op("dve", lambda e, pt_=pt_, i=i: e.tensor_tensor(pt_[:], pt_[:], dmask[:, i, :], ALU.mult),
                             reads=[pt_, dmask], writes=[pt_])
                if idx >= LA:
                    k2 = idx - LA
                    p, pt_ = slots[k2]
                    C.op("pe", lambda e, p=p, pt_=pt_, k2=k2: e.matmul(OPS[0:65, 0:128], VB[:, p, :], pt_[:], start=(k2 == 0), stop=(k2 == n - 1)),
                         reads=[VB, pt_], writes=[OPS])
            C.op("act", lambda e: e.copy(osb[0:65, :], OPS[0:65, 0:128]), reads=[OPS], writes=[osb])
            C.op("dve", lambda e: e.reciprocal(rec[64:65, :], osb[64:65, :]), reads=[osb], writes=[rec])
            C.op("pe", lambda e: e.matmul(BCP[0:64, 0:128], onesf[64:65, 0:64], rec[64:65, :], start=True, stop=True),
                 reads=[onesf, rec], writes=[BCP])
            C.op("dve", lambda e: e.tensor_tensor(OS[:, qs], osb[0:64, :], BCP[0:64, 0:128], ALU.mult), reads=[osb, BCP], writes=[OS])
        C.dma("sp", ot[h * 64:(h + 1) * 64, :], OS[:], reads=[OS], writes=[ot])
    return C.done()


def attn_consts(c):
    k = np.arange(128)[:, None]
    q = np.arange(128)[None, :]
    cm = np.zeros((8, 128, 128), np.float32)
    for i in range(8):
        if i == c:
            cm[i] = np.where(k <= q, 0.0, -30000.0)
        elif i > c:
            cm[i] = -30000.0
    dm = np.zeros((24, 128, 128), np.float32)
    for i in range(24):
        db = c + 16 - i
        if 0 <= db <= 16:
            dl = db * 128 + q - k
            m = ((dl >= 0) & (dl <= 128)).astype(np.float32)
            m += ((dl >= 0) & (dl <= 512) & (dl % 4 == 0))
            m += ((dl >= 0) & (dl <= 2048) & (dl % 16 == 0))
            dm[i] = m
    p = np.arange(128)
    g = 8 * (p % 16) + p // 16
    Pm = (g[:, None] // 2 == np.arange(64)[None, :]).astype(np.float32) / 256.0
    U = (g[:, None] < g[None, :]).astype(np.float32)
    selw = np.zeros((128, 16, 69), np.float32)
    for j in range(16):
        selw[c * 16 + j, j, 66] = 1.0
    own = (8 * np.arange(16) + c) // 2
    nn = np.arange(64)[None, :]
    pastneg = np.where(nn >= own[:, None], -1e30, 0.0).astype(np.float32)
    asel = np.where(nn < own[:, None], 30000.0, 0.0).astype(np.float32)
    bc = np.zeros((4, 16, 64), np.float32)
    b = 8 * np.arange(16) + c
    for h in range(4):
        bc[h] = ((nn == own[:, None]).astype(np.float32) - 1.0) * 30000.0 - MOBA_SLOPES[h] * 128.0 * (b[:, None] + 1)
    dilq = np.zeros((4, 2, NTOK), np.float32)
    for h in range(4):
        dilq[h, 0] = np.repeat(-DIL_SLOPES[h] * 128.0 * b, 128)
        dilq[h, 1] = np.tile(-DIL_SLOPES[h] * np.arange(128), 16)
    s = (g[None, :] * 128 + np.arange(128)[:, None]).astype(np.float32)
    kbs = np.stack([sl * s for sl in MOBA_SLOPES + DIL_SLOPES]).astype(np.float32)
    return dict(cmask=np.ascontiguousarray(cm.transpose(1, 0, 2)).astype(BFNP), dmask=np.ascontiguousarray(dm.transpose(1, 0, 2)).astype(BFNP),
                Pm=Pm, U=U, selw=selw.astype(BFNP), pastneg=pastneg.reshape(1, 1024), asel=asel.reshape(1, 1024),
                bc=bc.reshape(4, 1024), dilq=dilq.astype(BFNP), kbs=kbs)


_PROGS = {}


def _prog(name, fn):
    if name not in _PROGS:
        _PROGS[name] = fn()
    return _PROGS[name]


def kernel(**inputs):
    inp = {k: np.asarray(v) for k, v in inputs.items()}
    xcur = np.ascontiguousarray(inp["x"][0])
    cores = list(range(NCORE))
    for l in range(2):
        r1 = run_bass_kernel_spmd(_prog("pre", build_pre), pre_inputs(inp, l, xcur), core_ids=cores).results
        g = {k: np.stack([np.asarray(r1[c][k]) for c in cores]) for k in ("kt", "vv", "lf", "ks")}
        maps = []
        for c in cores:
            m = dict(qt=np.asarray(r1[c]["qt"]), qm=np.asarray(r1[c]["qm"]), ktg=g["kt"], vvg=g["vv"], lfg=g["lf"], ksg=g["ks"])
            m.update(attn_consts(c))
            maps.append(m)
        r2 = run_bass_kernel_spmd(_prog("attn", build_attn), maps, core_ids=cores).results
        r3 = run_bass_kernel_spmd(_prog("post", build_post), post_inputs(inp, l, xcur, [np.asarray(r2[c]["ot"]) for c in cores]),
                                  core_ids=cores).results
        xn = np.zeros((NB, NCORE, 128, D), np.float32)
        for c in cores:
            xn[:, c] = np.asarray(r3[c]["xo"])
        xcur = xn.reshape(SEQ, D)
    return xcur[None].astype(np.float32)
```
